# Optimizing a Trainium2 kernel written in Bass

```python
import math
import jax, jax.numpy as jnp
from jax import lax
import numpy as np

D_MODEL = 4096
BATCH = 2
SEQ = 4096
DEPTH = 2

N_META = 16
MIX_WIDTH = D_MODEL
W_HYENA = MIX_WIDTH // 4
W_FNET = MIX_WIDTH // 4
W_CONV = MIX_WIDTH // 4
W_ATTN = MIX_WIDTH - W_HYENA - W_FNET - W_CONV
HEAD_DIM = 128
N_Q_HEADS = W_ATTN // HEAD_DIM
N_KV_HEADS = 2
Q_PER_KV = N_Q_HEADS // N_KV_HEADS
KV_WIDTH = N_KV_HEADS * HEAD_DIM
WINDOW = 128
BLOCK = 128
FNET_GROUPS = 4
FNET_GROUP_W = W_FNET // FNET_GROUPS
SHORT_CONV = 3
DW_CONV = 31
FILTER_EMB = 33
FILTER_HIDDEN = 64
DECAY_MIN = -math.log(1e-2) / 1.5
DECAY_MAX = -math.log(1e-2) / 0.3
D_FF = 11008
N_EXPERTS = 8
TOP_K = 2
D_EXPERT = 4096
N_DENSE = (DEPTH + 1) // 2
N_MOE = DEPTH // 2
EPS = 1e-6
NEG = -1e30
IN_SIZES = [3 * W_HYENA, W_FNET, 2 * W_CONV, W_ATTN, KV_WIDTH, KV_WIDTH]
P_IN = sum(IN_SIZES)

kernel_name = 'hymba_style_hybrid_encoder'

F32 = jnp.float32


def _rmsnorm(x, g):
    xf = x.astype(F32)
    y = xf * lax.rsqrt(jnp.mean(xf * xf, axis=-1, keepdims=True) + EPS)
    return (y * g.astype(F32)).astype(x.dtype)


def _layernorm(x, g, b):
    xf = x.astype(F32)
    mu = jnp.mean(xf, axis=-1, keepdims=True)
    xc = xf - mu
    var = jnp.mean(xc * xc, axis=-1, keepdims=True)
    return (xc * lax.rsqrt(var + EPS) * g.astype(F32) + b.astype(F32)).astype(x.dtype)


def _depthwise_conv(u, w, b):
    K, C = w.shape
    y = lax.conv_general_dilated(u, w[:, None, :].astype(u.dtype), window_strides=(1,),
                                 padding=[(K // 2, K // 2)],
                                 dimension_numbers=('NWC', 'WIO', 'NWC'),
                                 feature_group_count=C)
    return y + b.astype(u.dtype)


def _hyena_filters(L, w1, b1, w2, b2, w3, b3, wo, freq, decay):
    t = jnp.linspace(0.0, 1.0, L, dtype=F32)[:, None]
    bands = (FILTER_EMB - 1) // 2
    w = 2.0 * math.pi * jnp.arange(L, dtype=F32)[:, None] / L
    f = jnp.linspace(1e-4, bands - 1, bands, dtype=F32)[None, :]
    z = jnp.concatenate([t, jnp.cos(f * w), -jnp.sin(f * w)], axis=-1)
    fr = freq.astype(F32)
    hid = jnp.sin(fr * (z @ w1.astype(F32) + b1.astype(F32)))
    hid = jnp.sin(fr * (hid @ w2.astype(F32) + b2.astype(F32)))
    hid = jnp.sin(fr * (hid @ w3.astype(F32) + b3.astype(F32)))
    h = (hid @ wo.astype(F32)).reshape(L, 2, W_HYENA)
    h = h * jnp.exp(-t[:, :, None] * decay.astype(F32)[None])
    return h[:, 0], h[:, 1]


def _bidir_long_conv(u, h_fwd, h_bwd):
    L, C = h_fwd.shape
    k = jnp.concatenate([h_fwd[:1] + h_bwd[:1], h_fwd[1:], jnp.zeros((1, C), F32),
                         h_bwd[1:][::-1]], axis=0)
    U = jnp.fft.rfft(u.astype(F32), n=2 * L, axis=1)
    Kf = jnp.fft.rfft(k, axis=0)
    y = jnp.fft.irfft(U * Kf[None], n=2 * L, axis=1)[:, :L]
    return y.astype(u.dtype)


def _hyena(u, conv_w, conv_b, h_fwd, h_bwd, skip):
    u = _depthwise_conv(u, conv_w, conv_b)
    x0, x1, v = jnp.split(u, 3, axis=-1)
    v = v * x1
    v = _bidir_long_conv(v, h_fwd, h_bwd) + skip.astype(v.dtype) * v
    return v * x0


def _fnet(u):
    B, L, C = u.shape
    ug = u.astype(F32).reshape(B, L, FNET_GROUPS, FNET_GROUP_W)
    y = jnp.fft.fft2(ug, axes=(1, 3), norm='ortho').real
    return y.reshape(B, L, C).astype(u.dtype)


def _conformer_conv(u, dw_w, dw_b, ln_g, ln_b):
    a, g = jnp.split(u, 2, axis=-1)
    y = a * jax.nn.sigmoid(g)
    y = _depthwise_conv(y, dw_w, dw_b)
    y = _layernorm(y, ln_g, ln_b)
    return jax.nn.silu(y)


def _alibi_slopes():
    s = np.array([2.0 ** (-8.0 * (h + 1) / N_Q_HEADS) for h in range(N_Q_HEADS)], dtype=np.float32)
    return jnp.asarray(s).reshape(N_KV_HEADS, Q_PER_KV)


def _dist_valid(qpos, kpos, kmeta, kin):
    delta = jnp.abs(qpos[:, :, None] - kpos[:, None, :])
    dist = jnp.where(kmeta[:, None, :], 0, delta).astype(F32)
    valid = kmeta[:, None, :] | (kin[:, None, :] & (delta <= WINDOW))
    return dist, valid


def _band_attend(q, k, v, dist, valid, sink):
    slopes = _alibi_slopes()
    s = jnp.einsum('bnqkgd,bnskd->bnkgqs', q, k).astype(F32) * (1.0 / math.sqrt(HEAD_DIM))
    s = s - slopes[:, :, None, None] * dist[:, None, None]
    s = jnp.where(valid[:, None, None], s, NEG)
    sk = sink.astype(F32).reshape(N_KV_HEADS, Q_PER_KV)[:, :, None, None]
    m = jnp.maximum(jnp.max(s, axis=-1, keepdims=True), sk)
    e = jnp.exp(s - m)
    p = e / (jnp.sum(e, axis=-1, keepdims=True) + jnp.exp(sk - m))
    return jnp.einsum('bnkgqs,bnskd->bnqkgd', p.astype(v.dtype), v)


def _windowed_gqa(q, k, v, sink):
    B, L = q.shape[0], q.shape[1]
    n = L - N_META
    nb = n // BLOCK
    qm, qr = q[:, :N_META], q[:, N_META:]
    km, kr = k[:, :N_META], k[:, N_META:]
    vm, vr = v[:, :N_META], v[:, N_META:]

    def band(t):
        tp = jnp.pad(t, ((0, 0), (BLOCK, BLOCK), (0, 0), (0, 0))).reshape(B, nb + 2, BLOCK, N_KV_HEADS, HEAD_DIM)
        return jnp.concatenate([tp[:, :-2], tp[:, 1:-1], tp[:, 2:]], axis=2)

    def with_meta(tm, tr):
        tmb = jnp.broadcast_to(tm[:, None], (B, nb, N_META, N_KV_HEADS, HEAD_DIM))
        return jnp.concatenate([tmb, band(tr)], axis=2)

    qb = qr.reshape(B, nb, BLOCK, N_KV_HEADS, Q_PER_KV, HEAD_DIM)
    kb, vb = with_meta(km, kr), with_meta(vm, vr)
    blk = jnp.arange(nb)[:, None]
    qpos = N_META + blk * BLOCK + jnp.arange(BLOCK)[None]
    kreal = (blk - 1) * BLOCK + jnp.arange(3 * BLOCK)[None]
    kpos = jnp.concatenate([jnp.broadcast_to(jnp.arange(N_META)[None], (nb, N_META)), N_META + kreal], axis=1)
    kmeta = jnp.concatenate([jnp.ones((nb, N_META), bool), jnp.zeros((nb, 3 * BLOCK), bool)], axis=1)
    kin = jnp.concatenate([jnp.ones((nb, N_META), bool), (kreal >= 0) & (kreal < n)], axis=1)
    dist, valid = _dist_valid(qpos, kpos, kmeta, kin)
    o_r = _band_attend(qb, kb, vb, dist, valid, sink).reshape(B, n, W_ATTN)

    qmb = qm.reshape(B, 1, N_META, N_KV_HEADS, Q_PER_KV, HEAD_DIM)
    kmb = jnp.concatenate([km, kr[:, :BLOCK]], axis=1)[:, None]
    vmb = jnp.concatenate([vm, vr[:, :BLOCK]], axis=1)[:, None]
    qpos_m = jnp.arange(N_META)[None]
    kpos_m = jnp.concatenate([jnp.arange(N_META), N_META + jnp.arange(BLOCK)])[None]
    kmeta_m = jnp.concatenate([jnp.ones((N_META,), bool), jnp.zeros((BLOCK,), bool)])[None]
    kin_m = jnp.ones((1, N_META + BLOCK), bool)
    dist_m, valid_m = _dist_valid(qpos_m, kpos_m, kmeta_m, kin_m)
    o_m = _band_attend(qmb, kmb, vmb, dist_m, valid_m, sink).reshape(B, N_META, W_ATTN)
    return jnp.concatenate([o_m, o_r], axis=1)


def _swiglu(x, w1, w3, w2):
    return (jax.nn.silu(x @ w1) * (x @ w3)) @ w2


def _moe(x, router_w, router_b, w1, w3, w2):
    logits = (x @ router_w).astype(F32) + router_b.astype(F32)
    top_v, top_i = lax.top_k(logits, TOP_K)
    top_w = jax.nn.softmax(top_v, axis=-1)
    gates = jnp.sum(jax.nn.one_hot(top_i, N_EXPERTS, dtype=F32) * top_w[..., None], axis=-2)
    y = jnp.zeros_like(x)
    for e in range(N_EXPERTS):
        y = y + gates[..., e:e + 1].astype(x.dtype) * _swiglu(x, w1[e], w3[e], w2[e])
    return y


def setup_inputs(seed: int = 0) -> dict:
    key = jax.random.key(seed)
    ks = iter(jax.random.split(key, 40))

    def nrm(shape, scale):
        return jax.random.normal(next(ks), shape, F32) * scale

    D = D_MODEL
    return {
        'x': nrm((BATCH, SEQ, D), 1.0),
        'meta_tokens': nrm((N_META, D), 1.0),
        'norm_mix_g': 1.0 + nrm((DEPTH, D), 0.02),
        'w_in': nrm((DEPTH, D, P_IN), D ** -0.5),
        'hy_conv_w': nrm((DEPTH, SHORT_CONV, 3 * W_HYENA), SHORT_CONV ** -0.5),
        'hy_conv_b': nrm((DEPTH, 3 * W_HYENA), 0.02),
        'hy_f_w1': nrm((DEPTH, FILTER_EMB, FILTER_HIDDEN), FILTER_EMB ** -0.5),
        'hy_f_b1': nrm((DEPTH, FILTER_HIDDEN), 0.02),
        'hy_f_w2': nrm((DEPTH, FILTER_HIDDEN, FILTER_HIDDEN), FILTER_HIDDEN ** -0.5),
        'hy_f_b2': nrm((DEPTH, FILTER_HIDDEN), 0.02),
        'hy_f_w3': nrm((DEPTH, FILTER_HIDDEN, FILTER_HIDDEN), FILTER_HIDDEN ** -0.5),
        'hy_f_b3': nrm((DEPTH, FILTER_HIDDEN), 0.02),
        'hy_f_wo': nrm((DEPTH, FILTER_HIDDEN, 2 * W_HYENA), FILTER_HIDDEN ** -0.5),
        'hy_f_freq': 1.0 + nrm((DEPTH, FILTER_HIDDEN), 0.02),
        'hy_decay': jax.random.uniform(next(ks), (DEPTH, 2, W_HYENA), F32, DECAY_MIN, DECAY_MAX),
        'hy_skip': nrm((DEPTH, W_HYENA), 0.5),
        'cv_dw_w': nrm((DEPTH, DW_CONV, W_CONV), DW_CONV ** -0.5),
        'cv_dw_b': nrm((DEPTH, W_CONV), 0.02),
        'cv_ln_g': 1.0 + nrm((DEPTH, W_CONV), 0.02),
        'cv_ln_b': nrm((DEPTH, W_CONV), 0.02),
        'attn_sink': nrm((DEPTH, N_Q_HEADS), 1.0),
        'group_norm_g': 1.0 + nrm((DEPTH, MIX_WIDTH), 0.02),
        'w_out': nrm((DEPTH, MIX_WIDTH, D), MIX_WIDTH ** -0.5),
        'norm_ffn_g': 1.0 + nrm((DEPTH, D), 0.02),
        'ffn_w1': nrm((N_DENSE, D, D_FF), D ** -0.5),
        'ffn_w3': nrm((N_DENSE, D, D_FF), D ** -0.5),
        'ffn_w2': nrm((N_DENSE, D_FF, D), D_FF ** -0.5),
        'router_w': nrm((N_MOE, D, N_EXPERTS), D ** -0.5),
        'router_b': nrm((N_MOE, N_EXPERTS), 0.01),
        'moe_w1': nrm((N_MOE, N_EXPERTS, D, D_EXPERT), D ** -0.5),
        'moe_w3': nrm((N_MOE, N_EXPERTS, D, D_EXPERT), D ** -0.5),
        'moe_w2': nrm((N_MOE, N_EXPERTS, D_EXPERT, D), D_EXPERT ** -0.5),
        'final_norm_g': 1.0 + nrm((D,), 0.02),
    }


def reference(x, meta_tokens, norm_mix_g, w_in, hy_conv_w, hy_conv_b, hy_f_w1, hy_f_b1, hy_f_w2,
              hy_f_b2, hy_f_w3, hy_f_b3, hy_f_wo, hy_f_freq, hy_decay, hy_skip, cv_dw_w, cv_dw_b,
              cv_ln_g, cv_ln_b, attn_sink, group_norm_g, w_out, norm_ffn_g, ffn_w1, ffn_w3, ffn_w2,
              router_w, router_b, moe_w1, moe_w3, moe_w2, final_norm_g):
    B = x.shape[0]
    h = jnp.concatenate([jnp.broadcast_to(meta_tokens[None].astype(x.dtype), (B, N_META, D_MODEL)), x], axis=1)
    L = h.shape[1]
    in_offsets = list(np.cumsum(IN_SIZES)[:-1])
    g_offsets = [W_HYENA, W_HYENA + W_FNET, W_HYENA + W_FNET + W_CONV]
    for l in range(DEPTH):
        xn = _rmsnorm(h, norm_mix_g[l])
        proj = xn @ w_in[l]
        u_a, u_b, u_c, q, k, v = jnp.split(proj, in_offsets, axis=-1)
        h_fwd, h_bwd = _hyena_filters(L, hy_f_w1[l], hy_f_b1[l], hy_f_w2[l], hy_f_b2[l], hy_f_w3[l],
                                      hy_f_b3[l], hy_f_wo[l], hy_f_freq[l], hy_decay[l])
        y_a = _hyena(u_a, hy_conv_w[l], hy_conv_b[l], h_fwd, h_bwd, hy_skip[l])
        y_b = _fnet(u_b)
        y_c = _conformer_conv(u_c, cv_dw_w[l], cv_dw_b[l], cv_ln_g[l], cv_ln_b[l])
        y_d = _windowed_gqa(q.reshape(B, L, N_Q_HEADS, HEAD_DIM), k.reshape(B, L, N_KV_HEADS, HEAD_DIM),
                            v.reshape(B, L, N_KV_HEADS, HEAD_DIM), attn_sink[l])
        g_a, g_b, g_c, g_d = jnp.split(group_norm_g[l], g_offsets)
        y = jnp.concatenate([_rmsnorm(y_a, g_a), _rmsnorm(y_b, g_b), _rmsnorm(y_c, g_c), _rmsnorm(y_d, g_d)], axis=-1)
        h = h + y @ w_out[l]
        xn = _rmsnorm(h, norm_ffn_g[l])
        if l % 2 == 0:
            i = l // 2
            h = h + _swiglu(xn, ffn_w1[i], ffn_w3[i], ffn_w2[i])
        else:
            i = l // 2
            h = h + _moe(xn, router_w[i], router_b[i], moe_w1[i], moe_w3[i], moe_w2[i])
    return _rmsnorm(h, final_norm_g)[:, N_META:]
```

```python
import math
import numpy as np
from contextlib import ExitStack
import ml_dtypes
import concourse.bass as bass
import concourse.mybir as mybir
from concourse.bass_utils import run_bass_kernel_spmd

F32 = mybir.dt.float32
BF16 = mybir.dt.bfloat16
AF = mybir.ActivationFunctionType
ALU = mybir.AluOpType
AX = mybir.AxisListType

ENGS = ["sync", "scalar", "vector", "gpsimd", "tensor"]
FLUSH_LIMIT = 6000

D = 4096
L = 4112
NM = 16
T = 1028
NTC = 4
KB = 32
EPS = 1e-6
PINP = 8192
DFF = 11008
NFB = 86
NE = 8
NF8 = 4113
N8 = 8224
PB_COLS = 92


class Sem:
    def __init__(self, h, step):
        self.h = h
        self.step = step
        self.count = 0


class Prog:
    def __init__(self, nc, stack):
        self.nc = nc
        self.stacks = [stack]
        self.q = {e: [] for e in ENGS}
        self.nsem = 0
        self.ninstr = 0

    @property
    def stack(self):
        return self.stacks[-1]

    def scope(self):
        prog = self

        class _Scope:
            def __enter__(s):
                prog.flush()
                st = ExitStack()
                st.__enter__()
                prog.stacks.append(st)
                return st

            def __exit__(s, *a):
                prog.flush()
                st = prog.stacks.pop()
                st.__exit__(None, None, None)
                return False

        return _Scope()

    def sem(self, name, dma=False):
        if not hasattr(self, "sempool"):
            self.sempool = {}
        if name in self.sempool:
            return self.sempool[name]
        h = self.stacks[0].enter_context(self.nc.semaphore(f"{name}_{self.nsem}"))
        self.nsem += 1
        sm = Sem(h, 16 if dma else 1)
        self.sempool[name] = sm
        return sm

    def sb(self, name, shape, dt):
        self.nsem += 1
        return self.stack.enter_context(self.nc.sbuf_tensor(f"{name}_{self.nsem}", shape, dt))

    def ps(self, name, shape, dt=F32):
        self.nsem += 1
        return self.stack.enter_context(self.nc.psum_tensor(f"{name}_{self.nsem}", shape, dt))

    def do(self, eng, fn, inc=None):
        ev = None
        if inc is not None:
            inc.count += inc.step
            ev = (inc, inc.count)
        self.q[eng].append(("op", fn, inc))
        self.ninstr += 1
        return ev

    def wait(self, eng, ev):
        if ev is None:
            return
        s, v = ev
        if v <= 0:
            return
        self.q[eng].append(("wait", s, v))

    def maybe_flush(self):
        if max(len(self.q[e]) for e in ENGS) > FLUSH_LIMIT:
            self.flush()

    def flush(self):
        if not any(self.q[e] for e in ENGS):
            return
        q = self.q
        self.q = {e: [] for e in ENGS}
        with self.nc.Block() as block:
            for e in ENGS:
                items = q[e]
                if not items:
                    continue

                def body(eng, items=items):
                    last_wait = {}
                    for it in items:
                        if it[0] == "wait":
                            _, s, v = it
                            if last_wait.get(id(s), 0) >= v:
                                continue
                            last_wait[id(s)] = v
                            eng.wait_ge(s.h, v)
                        else:
                            _, fn, inc = it
                            ins = fn(eng)
                            if inc is not None:
                                ins.then_inc(inc.h, inc.step)

                getattr(block, e)(body)


def chunks(total, size):
    out = []
    s = 0
    while s < total:
        out.append((s, min(size, total - s)))
        s += size
    return out


def tok_chunks(t=T):
    n = -(-t // 512)
    size = -(-t // n)
    return chunks(t, size)


class Ctx:
    def __init__(self, P):
        self.P = P
        self.psum = [P.ps(f"psb{i}", [128, 512]) for i in range(7)]
        self.psbf = P.ps("psbf", [128, 1024], BF16)
        self.ones32 = P.sb("ones32", [128, 128], F32)
        self.onesbf = P.sb("onesbf", [128, 128], BF16)
        self.identbf = P.sb("identbf", [128, 128], BF16)
        self.ident32 = P.sb("ident32", [128, 128], F32)
        self.pe = P.sem("pe")
        self.act = P.sem("act")
        self.dve = P.sem("dve")
        self.pool = P.sem("pool")
        self.dsy = P.sem("dsy", dma=True)
        self.dgp = P.sem("dgp", dma=True)
        self.last = None
        ev = P.do("gpsimd", lambda e: e.memset(self.ones32[:], 1.0), inc=self.pool)
        ev = P.do("gpsimd", lambda e: e.memset(self.onesbf[:], 1.0), inc=self.pool)
        P.wait("gpsimd", ev)
        self.last = ev
        self.base = (self.pe, self.act, self.dve, self.pool, self.dsy, self.dgp)
        P.flush()

    def renew(self):
        self.sync_chain()
        self.last = None

    def end_phase(self):
        self.sync_chain()
        self.last = None

    def load_ident(self, ident_dram):
        self.dma(lambda e: e.dma_start(out=self.ident32[:, :], in_=ident_dram))
        self.op("vector", lambda e: e.tensor_copy(out=self.identbf[:, :], in_=self.ident32[:, :]))
        self.sync_chain()

    def semof(self, eng):
        return {"tensor": self.pe, "scalar": self.act, "vector": self.dve, "gpsimd": self.pool}[eng]

    def op(self, eng, fn):
        P = self.P
        P.wait(eng, self.last)
        self.last = P.do(eng, fn, inc=self.semof(eng))
        P.maybe_flush()
        return self.last

    def dma(self, fn, eng="sync"):
        P = self.P
        P.wait(eng, self.last)
        self.last = P.do(eng, fn, inc=self.dsy if eng == "sync" else self.dgp)
        return self.last

    def mms(self, fns):
        P = self.P
        P.wait("tensor", self.last)
        for i, fn in enumerate(fns):
            ev = P.do("tensor", fn, inc=self.pe if i == len(fns) - 1 else None)
        self.last = ev
        P.maybe_flush()
        return ev

    def sync_chain(self):
        P = self.P
        for e in ENGS:
            P.wait(e, self.last)
        P.flush()


def load_small(cx, dst, src, eng="sync"):
    cx.dma(lambda e: e.dma_start(out=dst, in_=src), eng=eng)


def rmsnorm_T(cx, src_view, g_sb, dst, tag, nkb=KB, t=T, dsum=D, extra=None):
    P = cx.P
    hb = [P.sb(f"{tag}_hb{i}", [128, t], F32) for i in range(2)]
    sq = [P.sb(f"{tag}_sq{i}", [128, t], F32) for i in range(2)]
    rstd = P.sb(f"{tag}_rstd", [128, t], F32)
    ld = [P.sem(f"{tag}_ld{i}", dma=True) for i in range(2)]
    tcs = tok_chunks(t)
    pst = cx.psum[: len(tcs)]
    for e in ENGS:
        P.wait(e, cx.last)
    sq_free = [None, None]
    hb_free = [None, None]
    ev_last = None
    for kb in range(nkb):
        b = kb % 2
        P.wait("sync", hb_free[b])
        ev_ld = P.do("sync", lambda e, kb=kb, b=b: e.dma_start(out=hb[b][:], in_=src_view[:, kb, :]), inc=ld[b])
        P.wait("scalar", ev_ld)
        P.wait("scalar", sq_free[b])
        ev_sq = P.do("scalar", lambda e, b=b: e.activation(out=sq[b][:], in_=hb[b][:], func=AF.Square), inc=cx.act)
        hb_free[b] = ev_sq
        P.wait("tensor", ev_sq)
        for ci, (c0, cs) in enumerate(tcs):
            ev_mm = P.do(
                "tensor",
                lambda e, ci=ci, c0=c0, cs=cs, b=b, kb=kb: e.matmul(
                    pst[ci][:, :cs], cx.ones32[:], sq[b][:, c0 : c0 + cs], start=(kb == 0), stop=(kb == nkb - 1)
                ),
                inc=cx.pe if ci == len(tcs) - 1 else None,
            )
        sq_free[b] = ev_mm
        ev_last = ev_mm
    P.wait("scalar", ev_last)
    for ci, (c0, cs) in enumerate(tcs):
        ev = P.do(
            "scalar",
            lambda e, ci=ci, c0=c0, cs=cs: e.activation(
                out=rstd[:, c0 : c0 + cs], in_=pst[ci][:, :cs], func=AF.Sqrt, scale=1.0 / dsum, bias=EPS
            ),
            inc=cx.act,
        )
    P.wait("vector", ev)
    ev_r = P.do("vector", lambda e: e.reciprocal(out=rstd[:], in_=rstd[:]), inc=cx.dve)
    hb_free2 = [hb_free[0], hb_free[1]]
    evs = None
    for kb in range(nkb):
        b = kb % 2
        P.wait("sync", hb_free2[b])
        ev_ld = P.do("sync", lambda e, kb=kb, b=b: e.dma_start(out=hb[b][:], in_=src_view[:, kb, :]), inc=ld[b])
        P.wait("vector", ev_ld)
        P.wait("vector", ev_r)
        P.wait("vector", evs)
        evs = P.do(
            "vector",
            lambda e, kb=kb, b=b: e.scalar_tensor_tensor(
                out=dst[:, kb, :], in0=hb[b][:], scalar=g_sb[:, kb : kb + 1], in1=rstd[:], op0=ALU.mult, op1=ALU.mult
            ),
            inc=cx.dve,
        )
        hb_free2[b] = evs
        if extra is not None:
            hb_free2[b] = extra(kb, hb[b], ev_ld, evs)
    cx.last = evs
    for e in ENGS:
        P.wait(e, hb_free2[0])
        P.wait(e, hb_free2[1])
        P.wait(e, evs)
    P.flush()
    return rstd


def gemm_R(cx, wview, N, act, nkb, epilogue, tag, t=T, NW=512, psum_sets=None, wbufs=None):
    P = cx.P
    tcs = tok_chunks(t)
    ntc = len(tcs)
    if psum_sets is None:
        psum_sets = [cx.psum[0:ntc], cx.psum[ntc : 2 * ntc]]
    wb = wbufs if wbufs is not None else [P.sb(f"{tag}_w{i}", [128, nkb, NW], BF16) for i in range(2)]
    wld = [P.sem(f"{tag}_wld{i}", dma=True) for i in range(2)]
    for e in ENGS:
        P.wait(e, cx.last)
    wfree = [None, None]
    psfree = [None] * len(psum_sets)
    nbi = 0
    evf = None
    for wi, (n0, ns) in enumerate(chunks(N, NW)):
        b = wi % 2
        P.wait("gpsimd", wfree[b])
        ev_w = P.do("gpsimd", lambda e, b=b, n0=n0, ns=ns: e.dma_start(out=wb[b][:, :, :ns], in_=wview(n0, ns)), inc=wld[b])
        P.wait("tensor", ev_w)
        ev_mm = None
        for j0, js in chunks(ns, 128):
            nb = (n0 + j0) // 128
            si = nbi % len(psum_sets)
            pss = psum_sets[si]
            P.wait("tensor", psfree[si])
            for kb in range(nkb):
                for ci, (c0, cs) in enumerate(tcs):
                    last = kb == nkb - 1 and ci == ntc - 1
                    ev = P.do(
                        "tensor",
                        lambda e, b=b, kb=kb, j0=j0, js=js, ci=ci, c0=c0, cs=cs, pss=pss: e.matmul(
                            pss[ci][:js, :cs], wb[b][:, kb, j0 : j0 + js], act(kb, c0, cs),
                            start=(kb == 0), stop=(kb == nkb - 1)
                        ),
                        inc=cx.pe if last else None,
                    )
                    if last:
                        ev_mm = ev
            for ci, (c0, cs) in enumerate(tcs):
                evf = epilogue(nb, ci, pss[ci][:js, :cs], c0, cs, ev_mm, js)
            psfree[si] = evf
            nbi += 1
            P.maybe_flush()
        wfree[b] = ev_mm
    cx.last = evf
    return evf


def phaseA(cx, hsrc, g_sb, w_in, projT, c0tok):
    P = cx.P
    with P.scope():
        cx.renew()
        xnT = P.sb("A_xnT", [128, KB, T], BF16)
        with P.scope():
            rmsnorm_T(cx, hsrc[:, c0tok : c0tok + T].rearrange("(kb p) t -> p kb t", p=128), g_sb, xnT, "A_rn")
        with P.scope():
            stg = [P.sb(f"A_stg{i}", [128, T], F32) for i in range(3)]
            st = [P.sem(f"A_st{i}", dma=True) for i in range(3)]
            st_ev = [None] * 3
            wv = w_in.rearrange("(kb p) n -> p kb n", p=128)
            tcs = tok_chunks()

            def epi(nb, ci, ps, c0, cs, ev_mm, js):
                b = nb % 3
                eng = "scalar" if nb % 2 == 0 else "vector"
                sem = cx.act if eng == "scalar" else cx.dve
                P.wait(eng, ev_mm)
                if ci == 0:
                    P.wait(eng, st_ev[b])
                if eng == "scalar":
                    ev = P.do(eng, lambda e: e.copy(out=stg[b][:js, c0 : c0 + cs], in_=ps), inc=sem)
                else:
                    ev = P.do(eng, lambda e: e.tensor_copy(out=stg[b][:js, c0 : c0 + cs], in_=ps), inc=sem)
                if ci == len(tcs) - 1:
                    P.wait("sync", ev)
                    st_ev[b] = P.do(
                        "sync",
                        lambda e: e.dma_start(out=projT[nb * 128 : nb * 128 + js, c0tok : c0tok + T], in_=stg[b][:js, :]),
                        inc=st[b],
                    )
                return ev

            gemm_R(cx, lambda n0, ns: wv[:, :, n0 : n0 + ns], PINP, lambda kb, c0, cs: xnT[:, kb, c0 : c0 + cs], KB, epi, "A_g")
            for e in ENGS:
                for b in range(3):
                    P.wait(e, st_ev[b])
                P.wait(e, cx.last)
            cx.last = None
            P.flush()
        cx.end_phase()


NCH = 17


def blk_kk(blk, nvalid):
    return min(128, nvalid - blk * 128)


def transpose_blocks(cx, src_fn, n_list, dst_fn, psbf):
    i = 0
    while i < len(n_list):
        n = n_list[i]
        cnt = 1
        while i + cnt < len(n_list) and cnt < 8 and n_list[i + cnt] == n:
            cnt += 1
        fns = []
        for s in range(cnt):
            fns.append(lambda e, s=s, i=i, n=n: e.transpose(psbf[:n, s * 128 : (s + 1) * 128], src_fn(i + s), cx.identbf[:, :]))
        cx.mms(fns)
        cx.op("scalar", lambda e, i=i, cnt=cnt, n=n: e.copy(
            out=dst_fn(i, cnt, n), in_=psbf[:n, 0 : cnt * 128].rearrange("p (s c) -> p s c", c=128)))
        i += cnt


def load_table_chunk(cx, dst, tab, ch):
    cx.dma(lambda e: e.dma_start(out=dst[:, :, :], in_=tab[ch]), eng="sync")


def hyena_filters(cx, prm, j, hsum, hdiff):
    P = cx.P
    with P.scope():
        zT = P.sb("F_zT", [33, L], F32)
        hid = [P.sb(f"F_hid{i}", [64, L], F32) for i in range(2)]
        wsb = [P.sb("F_w1", [33, 64], F32), P.sb("F_w2", [64, 64], F32), P.sb("F_w3", [64, 64], F32)]
        wo = P.sb("F_wo", [64, 512], F32)
        fpar = P.sb("F_par", [64, 8], F32)
        decb = P.sb("F_dec", [128, 512], F32)
        negt = P.sb("F_negt", [128, 33], F32)
        tmp = P.sb("F_tmp", [128, 512], F32)
        hw = P.sb("F_hw", [128, 512], F32)
        npi = P.sb("F_npi", [128, 1], F32)
        load_small(cx, npi[:, :], prm["negpi"])
        load_small(cx, zT[:, :], prm["zT"])
        load_small(cx, wsb[0][:, :], prm["hy_w1"])
        load_small(cx, wsb[1][:, :], prm["hy_w2"])
        load_small(cx, wsb[2][:, :], prm["hy_w3"])
        load_small(cx, wo[:, 0:256], prm["hy_wo"][:, j * 256 : (j + 1) * 256])
        load_small(cx, wo[:, 256:512], prm["hy_wo"][:, 1024 + j * 256 : 1024 + (j + 1) * 256])
        load_small(cx, fpar[:, 0:4], prm["hy_fpar"])
        load_small(cx, negt[:, :], prm["negt"])
        load_small(cx, decb[:, 0:256], prm["hy_decay"][0:1, j * 256 : (j + 1) * 256].partition_broadcast(128))
        load_small(cx, decb[:, 256:512], prm["hy_decay"][1:2, j * 256 : (j + 1) * 256].partition_broadcast(128))
        s1 = P.sb("F_s1", [64, 512], F32)
        s2 = P.sb("F_s2", [64, 512], F32)
        cx.op("vector", lambda e: e.tensor_scalar(out=fpar[:, 7:8], in0=fpar[:, 0:1], scalar1=1.0 / 3.0, scalar2=None, op0=ALU.mult))
        for i in range(3):
            cx.op("vector", lambda e, i=i: e.tensor_tensor(out=fpar[:, 4 + i : 5 + i], in0=fpar[:, 7:8], in1=fpar[:, 1 + i : 2 + i], op=ALU.mult))
        src = zT
        kdim = 33
        for li in range(3):
            dst = hid[li % 2]
            for c0, cs in chunks(L, 512):
                ps = cx.psum[0]
                cx.mms([lambda e, c0=c0, cs=cs, li=li, src=src, kdim=kdim: e.matmul(
                    ps[:64, :cs], wsb[li][:kdim, :], src[:kdim, c0 : c0 + cs], start=True, stop=True)])
                cx.op("scalar", lambda e, cs=cs, li=li: e.activation(
                    out=s1[:, :cs], in_=ps[:64, :cs], func=AF.Sin, bias=fpar[:, 4 + li : 5 + li], scale=fpar[:, 7:8]))
                cx.op("vector", lambda e, cs=cs: e.tensor_tensor(out=s2[:, :cs], in0=s1[:, :cs], in1=s1[:, :cs], op=ALU.mult))
                cx.op("vector", lambda e, cs=cs: e.tensor_scalar(
                    out=s2[:, :cs], in0=s2[:, :cs], scalar1=-4.0, scalar2=3.0, op0=ALU.mult, op1=ALU.add))
                cx.op("vector", lambda e, c0=c0, cs=cs, dst=dst: e.tensor_tensor(
                    out=dst[:, c0 : c0 + cs], in0=s2[:, :cs], in1=s1[:, :cs], op=ALU.mult))
            src = dst
            kdim = 64
        for blk in range(33):
            kk = blk_kk(blk, L)
            ps = cx.psum[0]
            cx.mms([lambda e, blk=blk, kk=kk, src=src: e.matmul(
                ps[:kk, :512], src[:64, blk * 128 : blk * 128 + kk], wo[:, :], start=True, stop=True)])
            cx.op("vector", lambda e, blk=blk, kk=kk: e.tensor_scalar(
                out=tmp[:kk, :], in0=decb[:kk, :], scalar1=negt[:kk, blk : blk + 1], scalar2=None, op0=ALU.mult))
            cx.op("scalar", lambda e, kk=kk: e.activation(out=tmp[:kk, :], in_=tmp[:kk, :], func=AF.Exp))
            cx.op("vector", lambda e, kk=kk: e.tensor_tensor(out=hw[:kk, :], in0=ps[:kk, :512], in1=tmp[:kk, :], op=ALU.mult))
            cx.op("vector", lambda e, blk=blk, kk=kk: e.tensor_tensor(
                out=hsum[:kk, blk, :], in0=hw[:kk, 0:256], in1=hw[:kk, 256:512], op=ALU.add))
            cx.op("vector", lambda e, blk=blk, kk=kk: e.tensor_tensor(
                out=hdiff[:kk, blk, :], in0=hw[:kk, 256:512], in1=hw[:kk, 0:256], op=ALU.subtract))
        cx.sync_chain()


def hyena(cx, prm, j, projT, yT, parB):
    P = cx.P
    base = j * 2048
    hyscr = prm["hyscr"]
    psbf = cx.psbf
    with P.scope():
        cx.renew()
        hsum = P.sb("H_hsum", [128, 33, 256], BF16)
        hdiff = P.sb("H_hdiff", [128, 33, 256], BF16)
        vT = P.sb("H_vT", [128, 33, 256], BF16)
        YT = P.sb("H_YT", [128, 33, 2, 256], BF16)
        hyena_filters(cx, prm, j, hsum, hdiff)
        with P.scope():
            raw = P.sb("H_raw", [128, L + 2], F32)
            cv = [P.sb(f"H_cv{i}", [128, L], F32) for i in range(3)]
            vgbf = P.sb("H_vgbf", [128, L], BF16)
            cx.op("gpsimd", lambda e: e.memset(raw[:, 0:1], 0.0))
            cx.op("gpsimd", lambda e: e.memset(raw[:, L + 1 : L + 2], 0.0))
            for cb in range(2):
                for m in range(3):
                    r0 = base + m * 256 + cb * 128
                    cx.dma(lambda e, r0=r0: e.dma_start(out=raw[:, 1 : L + 1], in_=projT[r0 : r0 + 128, :]))
                    wc = (m * 2 + cb) * 3
                    bc = 18 + m * 2 + cb
                    cx.op("vector", lambda e, m=m, wc=wc, bc=bc: e.tensor_scalar(
                        out=cv[m][:, :], in0=raw[:, 0:L], scalar1=parB[:, wc : wc + 1], scalar2=parB[:, bc : bc + 1],
                        op0=ALU.mult, op1=ALU.add))
                    for k in (1, 2):
                        cx.op("vector", lambda e, m=m, wc=wc, k=k: e.scalar_tensor_tensor(
                            out=cv[m][:, :], in0=raw[:, k : k + L], scalar=parB[:, wc + k : wc + k + 1], in1=cv[m][:, :],
                            op0=ALU.mult, op1=ALU.add))
                cx.op("vector", lambda e: e.tensor_tensor(out=cv[2][:, :], in0=cv[2][:, :], in1=cv[1][:, :], op=ALU.mult))
                cx.op("vector", lambda e: e.tensor_copy(out=vgbf[:, :], in_=cv[2][:, :]))
                cx.dma(lambda e, cb=cb: e.dma_start(out=hyscr[(cb * 2) * 128 : (cb * 2 + 1) * 128, :], in_=cv[2][:, :]))
                cx.dma(lambda e, cb=cb: e.dma_start(out=hyscr[(cb * 2 + 1) * 128 : (cb * 2 + 2) * 128, :], in_=cv[0][:, :]))
                nl = [blk_kk(b, L) for b in range(33)]
                transpose_blocks(
                    cx, lambda i: vgbf[:, i * 128 : i * 128 + nl[i]], nl,
                    lambda i0, cnt, n, cb=cb: vT[:n, i0 : i0 + cnt, cb * 128 : (cb + 1) * 128], psbf)
            cx.sync_chain()
        with P.scope():
            Ct = P.sb("H_Ct", [128, 33, 256], BF16)
            St = P.sb("H_St", [128, 33, 256], BF16)
            ks = P.sb("H_ks", [128, 2, 256], F32)
            tt = [P.sb(f"H_tt{i}", [128, 256], F32) for i in range(2)]
            ybf = P.sb("H_ybf", [128, 2, 2, 256], BF16)
            for fc in range(NCH):
                wf = min(256, NF8 - fc * 256)
                load_table_chunk(cx, Ct, prm["C8"], fc)
                load_table_chunk(cx, St, prm["S8"], fc)
                fns = []
                for tg in range(8):
                    tab = Ct if tg < 4 else St
                    t4 = tg % 4
                    cbx = t4 % 2
                    if t4 < 2:
                        lh = vT
                    else:
                        lh = hsum if tg < 4 else hdiff
                    pst = cx.psum[tg // 2][:, (tg % 2) * 256 : (tg % 2) * 256 + wf]
                    for blk in range(33):
                        kk = blk_kk(blk, L)
                        fns.append(lambda e, tab=tab, lh=lh, cbx=cbx, pst=pst, blk=blk, kk=kk, wf=wf: e.matmul(
                            pst, lh[:kk, blk, cbx * 128 : (cbx + 1) * 128], tab[:kk, blk, :wf], start=(blk == 0), stop=(blk == 32)))
                cx.mms(fns)

                def tgt(tg, wf):
                    return cx.psum[tg // 2][:, (tg % 2) * 256 : (tg % 2) * 256 + wf]

                for cb in range(2):
                    cx.op("scalar", lambda e, cb=cb, wf=wf: e.mul(out=ks[:, 0, :wf], in_=tgt(2 + cb, wf), mul=2.0 / N8))
                    cx.op("scalar", lambda e, cb=cb, wf=wf: e.mul(out=ks[:, 1, :wf], in_=tgt(6 + cb, wf), mul=2.0 / N8))
                    if fc == 0:
                        cx.op("vector", lambda e: e.tensor_scalar(out=ks[:, :, 0:1], in0=ks[:, :, 0:1], scalar1=0.5, scalar2=None, op0=ALU.mult))
                    if fc == NCH - 1:
                        cx.op("vector", lambda e: e.tensor_scalar(out=ks[:, :, 16:17], in0=ks[:, :, 16:17], scalar1=0.5, scalar2=None, op0=ALU.mult))
                    cx.op("vector", lambda e, cb=cb, wf=wf: e.tensor_tensor(out=tt[0][:, :wf], in0=tgt(cb, wf), in1=ks[:, 0, :wf], op=ALU.mult))
                    cx.op("vector", lambda e, cb=cb, wf=wf: e.tensor_tensor(out=tt[1][:, :wf], in0=tgt(4 + cb, wf), in1=ks[:, 1, :wf], op=ALU.mult))
                    cx.op("vector", lambda e, cb=cb, wf=wf: e.tensor_tensor(out=ybf[:, cb, 0, :wf], in0=tt[0][:, :wf], in1=tt[1][:, :wf], op=ALU.add))
                    cx.op("vector", lambda e, cb=cb, wf=wf: e.tensor_tensor(out=tt[0][:, :wf], in0=tgt(cb, wf), in1=ks[:, 1, :wf], op=ALU.mult))
                    cx.op("vector", lambda e, cb=cb, wf=wf: e.tensor_tensor(out=tt[1][:, :wf], in0=tgt(4 + cb, wf), in1=ks[:, 0, :wf], op=ALU.mult))
                    cx.op("vector", lambda e, cb=cb, wf=wf: e.tensor_tensor(out=ybf[:, cb, 1, :wf], in0=tt[1][:, :wf], in1=tt[0][:, :wf], op=ALU.subtract))
                njj = 2 if wf == 256 else 1
                w = 128 if wf == 256 else wf
                fns = []
                for jj in range(njj):
                    for ri in range(2):
                        for cb in range(2):
                            slot = (jj * 2 + ri) * 2 + cb
                            fns.append(lambda e, jj=jj, ri=ri, cb=cb, slot=slot, w=w: e.transpose(
                                psbf[:w, slot * 128 : (slot + 1) * 128], ybf[:, cb, ri, jj * 128 : jj * 128 + w], cx.identbf[:, :]))
                cx.mms(fns)
                cx.op("scalar", lambda e, fc=fc, njj=njj, w=w: e.copy(
                    out=YT[:w, 2 * fc : 2 * fc + njj, :, :],
                    in_=psbf[:w, 0 : njj * 512].rearrange("p (a r c) -> p a r c", r=2, c=256)))
            cx.sync_chain()
        with P.scope():
            Ct = P.sb("H_Ct2", [128, 33, 256], BF16)
            St = P.sb("H_St2", [128, 33, 256], BF16)
            vgc = P.sb("H_vgc", [128, 256], F32)
            x0c = P.sb("H_x0c", [128, 256], F32)
            ot = P.sb("H_ot", [128, 256], F32)
            for tc in range(NCH):
                wt = min(256, L - tc * 256)
                t0 = tc * 256
                load_table_chunk(cx, Ct, prm["C8"], tc)
                load_table_chunk(cx, St, prm["S8"], tc)
                for cb in range(2):
                    fns = []
                    for ri in range(2):
                        tab = Ct if ri == 0 else St
                        for blk in range(33):
                            kk = blk_kk(blk, NF8)
                            fns.append(lambda e, tab=tab, ri=ri, blk=blk, kk=kk, cb=cb, wt=wt: e.matmul(
                                cx.psum[cb][:, :wt], YT[:kk, blk, ri, cb * 128 : (cb + 1) * 128], tab[:kk, blk, :wt],
                                start=(ri == 0 and blk == 0), stop=(ri == 1 and blk == 32)))
                    cx.mms(fns)
                    cx.dma(lambda e, cb=cb, t0=t0, wt=wt: e.dma_start(out=vgc[:, :wt], in_=hyscr[(cb * 2) * 128 : (cb * 2 + 1) * 128, t0 : t0 + wt]))
                    cx.dma(lambda e, cb=cb, t0=t0, wt=wt: e.dma_start(out=x0c[:, :wt], in_=hyscr[(cb * 2 + 1) * 128 : (cb * 2 + 2) * 128, t0 : t0 + wt]))
                    cx.op("vector", lambda e, cb=cb, wt=wt: e.scalar_tensor_tensor(
                        out=ot[:, :wt], in0=vgc[:, :wt], scalar=parB[:, 24 + cb : 25 + cb], in1=cx.psum[cb][:, :wt], op0=ALU.mult, op1=ALU.add))
                    cx.op("vector", lambda e, wt=wt: e.tensor_tensor(out=ot[:, :wt], in0=ot[:, :wt], in1=x0c[:, :wt], op=ALU.mult))
                    r0 = j * 256 + cb * 128
                    cx.dma(lambda e, r0=r0, t0=t0, wt=wt: e.dma_start(out=yT[r0 : r0 + 128, t0 : t0 + wt], in_=ot[:, :wt]))
            cx.sync_chain()
        cx.end_phase()


def fnet(cx, prm, j, projT, yT):
    P = cx.P
    base = j * 2048 + 768
    with P.scope():
        cx.renew()
        W1 = P.sb("N_W1", [128, 33, 256], BF16)
        W2n = P.sb("N_W2n", [128, 33, 256], BF16)
        with P.scope():
            u = P.sb("N_u", [128, L], F32)
            ubf = P.sb("N_ubf", [128, 2, L], BF16)
            cc = P.sb("N_cc", [128, 2, 256], BF16)
            sc = P.sb("N_sc", [128, 2, 256], BF16)
            load_small(cx, cc[:, :, :], prm["CC"])
            load_small(cx, sc[:, :, :], prm["SC"])
            for cb in range(2):
                cx.dma(lambda e, cb=cb: e.dma_start(out=u[:, :], in_=projT[base + cb * 128 : base + (cb + 1) * 128, :]))
                cx.op("vector", lambda e, cb=cb: e.tensor_copy(out=ubf[:, cb, :], in_=u[:, :]))
            for blk in range(33):
                kk = blk_kk(blk, L)
                ps = cx.psum[0]
                fns = []
                for half, tab in enumerate((cc, sc)):
                    for cb in range(2):
                        fns.append(lambda e, half=half, tab=tab, cb=cb, blk=blk, kk=kk: e.matmul(
                            ps[:kk, half * 256 : (half + 1) * 256], ubf[:, cb, blk * 128 : blk * 128 + kk], tab[:, cb, :],
                            start=(cb == 0), stop=(cb == 1)))
                cx.mms(fns)
                cx.op("scalar", lambda e, blk=blk, kk=kk: e.copy(out=W1[:kk, blk, :], in_=ps[:kk, 0:256]))
                cx.op("vector", lambda e, blk=blk, kk=kk: e.tensor_scalar(
                    out=W2n[:kk, blk, :], in0=ps[:kk, 256:512], scalar1=-1.0, scalar2=None, op0=ALU.mult))
            cx.sync_chain()
        with P.scope():
            Ct = P.sb("N_Ct", [128, 33, 256], BF16)
            St = P.sb("N_St", [128, 33, 256], BF16)
            ot = P.sb("N_ot", [128, 256], F32)
            sc_o = 1.0 / math.sqrt(L * 256.0)
            for pc in range(NCH):
                wp = min(256, L - pc * 256)
                p0 = pc * 256
                load_table_chunk(cx, Ct, prm["C4"], pc)
                load_table_chunk(cx, St, prm["S4"], pc)
                for qb in range(2):
                    fns = []
                    for ri in range(2):
                        tab = Ct if ri == 0 else St
                        lh = W1 if ri == 0 else W2n
                        for blk in range(33):
                            kk = blk_kk(blk, L)
                            fns.append(lambda e, tab=tab, lh=lh, ri=ri, blk=blk, kk=kk, qb=qb, wp=wp: e.matmul(
                                cx.psum[qb][:, :wp], lh[:kk, blk, qb * 128 : (qb + 1) * 128], tab[:kk, blk, :wp],
                                start=(ri == 0 and blk == 0), stop=(ri == 1 and blk == 32)))
                    cx.mms(fns)
                    cx.op("scalar", lambda e, qb=qb, wp=wp: e.mul(out=ot[:, :wp], in_=cx.psum[qb][:, :wp], mul=sc_o))
                    r0 = 1024 + j * 256 + qb * 128
                    cx.dma(lambda e, r0=r0, p0=p0, wp=wp: e.dma_start(out=yT[r0 : r0 + 128, p0 : p0 + wp], in_=ot[:, :wp]))
            cx.sync_chain()
        cx.end_phase()


def confconv(cx, prm, j, projT, yT, parB):
    P = cx.P
    base = j * 2048 + 1024
    with P.scope():
        cx.renew()
        a = P.sb("V_a", [128, L], F32)
        g = P.sb("V_g", [128, L], F32)
        ypad = P.sb("V_ypad", [128, L + 30], F32)
        acc = P.sb("V_acc", [128, L], F32)
        cx.op("gpsimd", lambda e: e.memset(ypad[:, 0:15], 0.0))
        cx.op("gpsimd", lambda e: e.memset(ypad[:, L + 15 : L + 30], 0.0))
        for cb in range(2):
            cx.dma(lambda e, cb=cb: e.dma_start(out=a[:, :], in_=projT[base + cb * 128 : base + (cb + 1) * 128, :]))
            cx.dma(lambda e, cb=cb: e.dma_start(out=g[:, :], in_=projT[base + 256 + cb * 128 : base + 256 + (cb + 1) * 128, :]))
            cx.op("scalar", lambda e: e.activation(out=g[:, :], in_=g[:, :], func=AF.Sigmoid))
            cx.op("vector", lambda e: e.tensor_tensor(out=ypad[:, 15 : 15 + L], in0=a[:, :], in1=g[:, :], op=ALU.mult))
            wc = 26 + cb * 31
            bc = 88 + cb
            cx.op("vector", lambda e, wc=wc, bc=bc: e.tensor_scalar(
                out=acc[:, :], in0=ypad[:, 0:L], scalar1=parB[:, wc : wc + 1], scalar2=parB[:, bc : bc + 1], op0=ALU.mult, op1=ALU.add))
            for k in range(1, 31):
                cx.op("vector", lambda e, wc=wc, k=k: e.scalar_tensor_tensor(
                    out=acc[:, :], in0=ypad[:, k : k + L], scalar=parB[:, wc + k : wc + k + 1], in1=acc[:, :], op0=ALU.mult, op1=ALU.add))
            r0 = 2048 + j * 256 + cb * 128
            cx.dma(lambda e, r0=r0: e.dma_start(out=yT[r0 : r0 + 128, :], in_=acc[:, :]))
        cx.end_phase()


def attention(cx, prm, j, projT, yT, parB):
    P = cx.P
    base = j * 2048
    psbf = cx.psbf
    scale = 1.0 / math.sqrt(128.0)
    with P.scope():
        cx.renew()
        qbf = P.sb("T_q", [128, 2, L], BF16)
        kbf = P.sb("T_k", [128, L], BF16)
        vbf = P.sb("T_v", [128, L], BF16)
        vr = P.sb("T_vr", [128, 32, 128], BF16)
        vmeta = P.sb("T_vm", [16, 128], BF16)
        ld = P.sb("T_ld", [128, L], F32)
        bias = P.sb("T_bias", [128, 2, 384], F32)
        biasm = P.sb("T_biasm", [128, 2, 16], F32)
        esink = P.sb("T_esink", [128, 2], F32)
        tmp = P.sb("T_tmp", [128, 512], F32)
        pb = P.sb("T_pb", [128, 512], BF16)
        den = P.sb("T_den", [128, 512], F32)
        ot = P.sb("T_ot", [128, 512], F32)
        load_small(cx, bias[:, :, :], prm["attn_bias"][j])
        load_small(cx, biasm[:, :, :], prm["attn_biasm"][j])
        cx.op("scalar", lambda e: e.activation(out=esink[:, :], in_=parB[:, 90:92], func=AF.Exp))
        for hh in range(2):
            r0 = base + 1536 + hh * 128
            cx.dma(lambda e, r0=r0: e.dma_start(out=ld[:, :], in_=projT[r0 : r0 + 128, :]))
            cx.op("vector", lambda e, hh=hh: e.tensor_copy(out=qbf[:, hh, :], in_=ld[:, :]))
        cx.dma(lambda e: e.dma_start(out=ld[:, :], in_=projT[base + 1792 : base + 1920, :]))
        cx.op("vector", lambda e: e.tensor_copy(out=kbf[:, :], in_=ld[:, :]))
        cx.dma(lambda e: e.dma_start(out=ld[:, :], in_=projT[base + 1920 : base + 2048, :]))
        cx.op("vector", lambda e: e.tensor_copy(out=vbf[:, :], in_=ld[:, :]))
        transpose_blocks(cx, lambda i: vbf[:, 0:16], [16], lambda i0, cnt, n: vmeta[:n, :].rearrange("p (s c) -> p s c", s=1), psbf)
        transpose_blocks(cx, lambda i: vbf[:, 16 + i * 128 : 16 + (i + 1) * 128], [128] * 32,
                         lambda i0, cnt, n: vr[:n, i0 : i0 + cnt, :], psbf)
        ops_, dps, sps = cx.psum[0], cx.psum[1], cx.psum[2]
        for hh in range(2):
            rout = 3072 + j * 256 + hh * 128
            for qc in range(-1, 8):
                if qc < 0:
                    nq = 16
                    qlo = 0
                    cx.mms([lambda e, hh=hh: e.matmul(sps[:16, 0:16], kbf[:, 0:16], qbf[:, hh, 0:16], start=True, stop=True),
                            lambda e, hh=hh: e.matmul(sps[:, 32:48], kbf[:, 16:144], qbf[:, hh, 0:16], start=True, stop=True)])
                    cx.op("scalar", lambda e: e.activation(out=pb[:16, 0:16], in_=sps[:16, 0:16], func=AF.Exp, scale=scale))
                    cx.op("vector", lambda e, hh=hh: e.scalar_tensor_tensor(
                        out=tmp[:, 0:16], in0=sps[:, 32:48], scalar=scale, in1=biasm[:, hh, :], op0=ALU.mult, op1=ALU.add))
                    cx.op("scalar", lambda e: e.activation(out=pb[:, 32:48], in_=tmp[:, 0:16], func=AF.Exp))
                    cx.mms([lambda e: e.matmul(ops_[:, 0:16], vmeta[:16, :], pb[:16, 0:16], start=True, stop=False),
                            lambda e: e.matmul(ops_[:, 0:16], vr[:, 0, :], pb[:, 32:48], start=False, stop=True),
                            lambda e: e.matmul(dps[:, 0:16], cx.onesbf[:16, :], pb[:16, 0:16], start=True, stop=False),
                            lambda e: e.matmul(dps[:, 0:16], cx.onesbf[:, :], pb[:, 32:48], start=False, stop=True)])
                else:
                    nq = 512
                    qlo = 16 + 512 * qc
                    cx.mms([lambda e, hh=hh, qlo=qlo: e.matmul(sps[:16, :512], kbf[:, 0:16], qbf[:, hh, qlo : qlo + 512], start=True, stop=True)])
                    cx.op("scalar", lambda e: e.activation(out=pb[:16, :512], in_=sps[:16, :512], func=AF.Exp, scale=scale))
                    cx.mms([lambda e: e.matmul(ops_[:, :512], vmeta[:16, :], pb[:16, :512], start=True, stop=False),
                            lambda e: e.matmul(dps[:, :512], cx.onesbf[:16, :], pb[:16, :512], start=True, stop=False)])
                    for kb in range(4 * qc - 1, 4 * qc + 5):
                        if kb < 0 or kb >= 32:
                            continue
                        qb_lo = max(kb - 1, 4 * qc)
                        qb_hi = min(kb + 1, 4 * qc + 3)
                        w = (qb_hi - qb_lo + 1) * 128
                        c0 = (qb_lo - 4 * qc) * 128
                        rel = qb_lo - (kb - 1)
                        cx.mms([lambda e, hh=hh, kb=kb, qb_lo=qb_lo, w=w: e.matmul(
                            sps[:, :w], kbf[:, 16 + 128 * kb : 16 + 128 * (kb + 1)], qbf[:, hh, 16 + 128 * qb_lo : 16 + 128 * qb_lo + w],
                            start=True, stop=True)])
                        cx.op("vector", lambda e, hh=hh, rel=rel, w=w: e.scalar_tensor_tensor(
                            out=tmp[:, :w], in0=sps[:, :w], scalar=scale, in1=bias[:, hh, rel * 128 : rel * 128 + w], op0=ALU.mult, op1=ALU.add))
                        cx.op("scalar", lambda e, w=w: e.activation(out=pb[:, :w], in_=tmp[:, :w], func=AF.Exp))
                        cx.mms([lambda e, kb=kb, c0=c0, w=w: e.matmul(ops_[:, c0 : c0 + w], vr[:, kb, :], pb[:, :w], start=False, stop=False),
                                lambda e, c0=c0, w=w: e.matmul(dps[:, c0 : c0 + w], cx.onesbf[:, :], pb[:, :w], start=False, stop=False)])
                cx.op("vector", lambda e, hh=hh, nq=nq: e.tensor_scalar(
                    out=den[:, :nq], in0=dps[:, :nq], scalar1=esink[:, hh : hh + 1], scalar2=None, op0=ALU.add))
                cx.op("vector", lambda e, nq=nq: e.reciprocal(out=den[:, :nq], in_=den[:, :nq]))
                cx.op("vector", lambda e, nq=nq: e.tensor_tensor(out=ot[:, :nq], in0=ops_[:, :nq], in1=den[:, :nq], op=ALU.mult))
                cx.dma(lambda e, rout=rout, qlo=qlo, nq=nq: e.dma_start(out=yT[rout : rout + 128, qlo : qlo + nq], in_=ot[:, :nq]))
        cx.end_phase()


def phaseB(cx, prm, projT, yT, which=("hy", "fn", "cv", "at"), slices=(0, 1, 2, 3)):
    P = cx.P
    with P.scope():
        parB = P.sb("B_par", [128, PB_COLS], F32)
        for j in slices:
            load_small(cx, parB[:, :], prm["parB"][:, j, :])
            cx.sync_chain()
            if "hy" in which:
                hyena(cx, prm, j, projT, yT, parB)
            if "fn" in which:
                fnet(cx, prm, j, projT, yT)
            if "cv" in which:
                confconv(cx, prm, j, projT, yT, parB)
            if "at" in which:
                attention(cx, prm, j, projT, yT, parB)


_CONST = {}


def _tile_table(tab):
    R, Cc = tab.shape
    full = np.zeros((33 * 128, NCH * 256), np.float32)
    full[:R, :Cc] = tab
    out = full.reshape(33, 128, NCH, 256).transpose(2, 1, 0, 3)
    return np.ascontiguousarray(out).astype(ml_dtypes.bfloat16)


def constants():
    if _CONST:
        return _CONST
    c = _CONST
    a = np.arange(NF8, dtype=np.int64)
    m8 = (a[:, None] * a[None, :]) % N8
    ang = 2.0 * np.pi * m8.astype(np.float64) / N8
    c["C8"] = _tile_table(np.cos(ang).astype(np.float32))
    c["S8"] = _tile_table(np.sin(ang).astype(np.float32))
    a4 = np.arange(L, dtype=np.int64)
    m4 = (a4[:, None] * a4[None, :]) % L
    ang = 2.0 * np.pi * m4.astype(np.float64) / L
    c["C4"] = _tile_table(np.cos(ang).astype(np.float32))
    c["S4"] = _tile_table(np.sin(ang).astype(np.float32))
    del ang, m8, m4
    q = np.arange(256, dtype=np.int64)
    angc = 2.0 * np.pi * ((q[:, None] * q[None, :]) % 256).astype(np.float64) / 256.0
    cc = np.cos(angc).astype(np.float32).reshape(2, 128, 256).transpose(1, 0, 2)
    sc = np.sin(angc).astype(np.float32).reshape(2, 128, 256).transpose(1, 0, 2)
    c["CC"] = np.ascontiguousarray(cc).astype(ml_dtypes.bfloat16)
    c["SC"] = np.ascontiguousarray(sc).astype(ml_dtypes.bfloat16)
    t = np.linspace(0.0, 1.0, L, dtype=np.float32)[:, None]
    w = (2.0 * math.pi * np.arange(L, dtype=np.float32)[:, None] / L).astype(np.float32)
    f = np.linspace(1e-4, 15, 16, dtype=np.float32)[None, :]
    z = np.concatenate([t, np.cos(f * w), -np.sin(f * w)], axis=-1).astype(np.float32)
    c["zT"] = np.ascontiguousarray(z.T)
    tt = np.zeros(33 * 128, np.float32)
    tt[:L] = t[:, 0]
    c["negt"] = np.ascontiguousarray(-tt.reshape(33, 128).T)
    c["negpi"] = np.full((128, 1), -math.pi, np.float32)
    c["ident"] = np.eye(128, dtype=np.float32)
    NEG = -1e30
    p = np.arange(128)[:, None]
    cidx = np.arange(384)[None, :]
    delta = np.abs(cidx - 128 - p)
    bias = np.zeros((4, 128, 2, 384), np.float32)
    biasm = np.zeros((4, 128, 2, 16), np.float32)
    qm = np.arange(16)[None, :]
    dm = 16 + p - qm
    for j in range(4):
        for hh in range(2):
            h = 2 * j + hh
            slope = np.float32(2.0 ** (-8.0 * (h + 1) / 8))
            bias[j, :, hh, :] = np.where(delta <= 128, -slope * delta.astype(np.float32), NEG)
            biasm[j, :, hh, :] = np.where(dm <= 128, -slope * dm.astype(np.float32), NEG)
    c["attn_bias"] = bias
    c["attn_biasm"] = biasm
    perm = []
    for j in range(4):
        s = j * 256
        perm += list(range(0 + s, 0 + s + 256))
        perm += list(range(1024 + s, 1024 + s + 256))
        perm += list(range(2048 + s, 2048 + s + 256))
        perm += list(range(3072 + s, 3072 + s + 256))
        perm += list(range(4096 + s, 4096 + s + 256))
        perm += list(range(5120 + s, 5120 + s + 256))
        perm += list(range(6144 + s, 6144 + s + 256))
        kv = j // 2
        perm += list(range(7168 + kv * 128, 7168 + kv * 128 + 128))
        perm += list(range(7424 + kv * 128, 7424 + kv * 128 + 128))
    c["perm"] = np.asarray(perm, np.int64)
    return c


def layer_params(inp, l):
    out = {}
    parB = np.zeros((128, 4, PB_COLS), np.float32)
    cw = inp["hy_conv_w"][l]
    cbias = inp["hy_conv_b"][l]
    for j in range(4):
        for m in range(3):
            for cb in range(2):
                ch = m * 1024 + j * 256 + cb * 128
                for k in range(3):
                    parB[:, j, (m * 2 + cb) * 3 + k] = cw[k, ch : ch + 128]
                parB[:, j, 18 + m * 2 + cb] = cbias[ch : ch + 128]
        for cb in range(2):
            ch = j * 256 + cb * 128
            parB[:, j, 24 + cb] = inp["hy_skip"][l][ch : ch + 128]
            parB[:, j, 26 + cb * 31 : 26 + cb * 31 + 31] = inp["cv_dw_w"][l][:, ch : ch + 128].T
            parB[:, j, 88 + cb] = inp["cv_dw_b"][l][ch : ch + 128]
        for hh in range(2):
            parB[:, j, 90 + hh] = inp["attn_sink"][l][2 * j + hh]
    out["parB"] = parB
    out["hy_w1"] = np.ascontiguousarray(inp["hy_f_w1"][l])
    out["hy_w2"] = np.ascontiguousarray(inp["hy_f_w2"][l])
    out["hy_w3"] = np.ascontiguousarray(inp["hy_f_w3"][l])
    out["hy_wo"] = np.ascontiguousarray(inp["hy_f_wo"][l])
    out["hy_fpar"] = np.ascontiguousarray(
        np.stack([inp["hy_f_freq"][l], inp["hy_f_b1"][l], inp["hy_f_b2"][l], inp["hy_f_b3"][l]], axis=1))
    out["hy_decay"] = np.ascontiguousarray(inp["hy_decay"][l])

    def pk(v):
        return np.ascontiguousarray(v.reshape(-1, 128).T)

    parC = np.concatenate(
        [pk(inp["norm_mix_g"][l]), pk(inp["group_norm_g"][l]), pk(inp["norm_ffn_g"][l]),
         pk(inp["cv_ln_g"][l]), pk(inp["cv_ln_b"][l]), pk(inp["final_norm_g"])], axis=1)
    out["parC"] = np.ascontiguousarray(parC.astype(np.float32))
    return out


def sumsq_to_rstd(cx, blocks_fn, nblk, rstd, sqt, dsum, banks=(0, 1, 2)):
    tcs = tok_chunks()
    for kb in range(nblk):
        cx.op("scalar", lambda e, kb=kb: e.activation(out=sqt[:, :], in_=blocks_fn(kb), func=AF.Square))
        cx.mms([lambda e, kb=kb, ci=ci, c0=c0, cs=cs: e.matmul(
            cx.psum[banks[ci]][:, :cs], cx.ones32[:, :], sqt[:, c0 : c0 + cs], start=(kb == 0), stop=(kb == nblk - 1))
            for ci, (c0, cs) in enumerate(tcs)])
    for ci, (c0, cs) in enumerate(tcs):
        cx.op("scalar", lambda e, ci=ci, c0=c0, cs=cs: e.activation(
            out=rstd[:, c0 : c0 + cs], in_=cx.psum[banks[ci]][:, :cs], func=AF.Sqrt, scale=1.0 / dsum, bias=EPS))
    cx.op("vector", lambda e: e.reciprocal(out=rstd[:, :], in_=rstd[:, :]))


def phaseC_norms(cx, yT, parC, yn, c0tok):
    P = cx.P
    tcs = tok_chunks()
    with P.scope():
        xg = P.sb("C_xg", [128, 8, T], F32)
        sqt = P.sb("C_sqt", [128, T], F32)
        mean = P.sb("C_mean", [128, T], F32)
        rstd = P.sb("C_rstd", [128, T], F32)
        tt = P.sb("C_tt", [128, T], F32)
        sg = P.sb("C_sg", [128, T], F32)
        for gi in range(4):
            cx.dma(lambda e, gi=gi: e.dma_start(
                out=xg[:, :, :], in_=yT[gi * 1024 : (gi + 1) * 1024, c0tok : c0tok + T].rearrange("(kb p) t -> p kb t", p=128)))
            if gi == 2:
                for kb in range(8):
                    cx.mms([lambda e, kb=kb, ci=ci, c0=c0, cs=cs: e.matmul(
                        cx.psum[ci][:, :cs], cx.ones32[:, :], xg[:, kb, c0 : c0 + cs], start=(kb == 0), stop=(kb == 7))
                        for ci, (c0, cs) in enumerate(tcs)])
                for kb in range(8):
                    cx.op("scalar", lambda e, kb=kb: e.activation(out=sqt[:, :], in_=xg[:, kb, :], func=AF.Square))
                    cx.mms([lambda e, kb=kb, ci=ci, c0=c0, cs=cs: e.matmul(
                        cx.psum[3 + ci][:, :cs], cx.ones32[:, :], sqt[:, c0 : c0 + cs], start=(kb == 0), stop=(kb == 7))
                        for ci, (c0, cs) in enumerate(tcs)])
                for ci, (c0, cs) in enumerate(tcs):
                    cx.op("scalar", lambda e, ci=ci, c0=c0, cs=cs: e.mul(out=mean[:, c0 : c0 + cs], in_=cx.psum[ci][:, :cs], mul=1.0 / 1024.0))
                    cx.op("vector", lambda e, c0=c0, cs=cs: e.tensor_tensor(
                        out=sqt[:, c0 : c0 + cs], in0=mean[:, c0 : c0 + cs], in1=mean[:, c0 : c0 + cs], op=ALU.mult))
                    cx.op("vector", lambda e, ci=ci, c0=c0, cs=cs: e.scalar_tensor_tensor(
                        out=rstd[:, c0 : c0 + cs], in0=cx.psum[3 + ci][:, :cs], scalar=1.0 / 1024.0, in1=sqt[:, c0 : c0 + cs],
                        op0=ALU.mult, op1=ALU.subtract))
                cx.op("scalar", lambda e: e.activation(out=rstd[:, :], in_=rstd[:, :], func=AF.Sqrt, scale=1.0, bias=EPS))
                cx.op("vector", lambda e: e.reciprocal(out=rstd[:, :], in_=rstd[:, :]))
                for kb in range(8):
                    cx.op("vector", lambda e, kb=kb: e.tensor_tensor(out=tt[:, :], in0=xg[:, kb, :], in1=mean[:, :], op=ALU.subtract))
                    cx.op("vector", lambda e: e.tensor_tensor(out=tt[:, :], in0=tt[:, :], in1=rstd[:, :], op=ALU.mult))
                    cx.op("vector", lambda e, kb=kb: e.tensor_scalar(
                        out=tt[:, :], in0=tt[:, :], scalar1=parC[:, 96 + kb : 97 + kb], scalar2=parC[:, 104 + kb : 105 + kb],
                        op0=ALU.mult, op1=ALU.add))
                    cx.op("scalar", lambda e: e.activation(out=sg[:, :], in_=tt[:, :], func=AF.Sigmoid))
                    cx.op("vector", lambda e, kb=kb: e.tensor_tensor(out=xg[:, kb, :], in0=tt[:, :], in1=sg[:, :], op=ALU.mult))
            sumsq_to_rstd(cx, lambda kb: xg[:, kb, :], 8, rstd, sqt, 1024.0)
            for kb in range(8):
                cx.op("vector", lambda e, kb=kb, gi=gi: e.scalar_tensor_tensor(
                    out=yn[:, gi * 8 + kb, :], in0=xg[:, kb, :], scalar=parC[:, 32 + gi * 8 + kb : 33 + gi * 8 + kb], in1=rstd[:, :],
                    op0=ALU.mult, op1=ALU.mult))
        cx.sync_chain()


def make_rmw_epi(cx, hin, hout, c0tok, tag):
    P = cx.P
    hres = [P.sb(f"{tag}_hres{i}", [128, T], F32) for i in range(3)]
    ld = [P.sem(f"{tag}_ld{i}", dma=True) for i in range(3)]
    st = [P.sem(f"{tag}_st{i}", dma=True) for i in range(3)]
    state = {"st_ev": [None] * 3, "ld_ev": [None] * 3}
    ntc = len(tok_chunks())

    def epi(nb, ci, ps, c0, cs, ev_mm, js):
        b = nb % 3
        if ci == 0:
            P.wait("sync", state["st_ev"][b])
            state["ld_ev"][b] = P.do(
                "sync", lambda e: e.dma_start(out=hres[b][:js, :], in_=hin[nb * 128 : nb * 128 + js, c0tok : c0tok + T]), inc=ld[b])
        P.wait("vector", ev_mm)
        P.wait("vector", state["ld_ev"][b])
        ev = P.do("vector", lambda e: e.tensor_tensor(
            out=hres[b][:js, c0 : c0 + cs], in0=ps, in1=hres[b][:js, c0 : c0 + cs], op=ALU.add), inc=cx.dve)
        if ci == ntc - 1:
            P.wait("sync", ev)
            state["st_ev"][b] = P.do(
                "sync", lambda e: e.dma_start(out=hout[nb * 128 : nb * 128 + js, c0tok : c0tok + T], in_=hres[b][:js, :]), inc=st[b])
        return ev

    def finish():
        for e in ENGS:
            for b in range(3):
                P.wait(e, state["st_ev"][b])
            P.wait(e, cx.last)
        cx.last = None
        P.flush()

    return epi, finish


def wv_of(w, r0, nkb):
    v = w[r0 : r0 + nkb * 128, :].rearrange("(kb p) n -> p kb n", p=128)
    return lambda n0, ns: v[:, :, n0 : n0 + ns]


def phaseC_wout(cx, w_out, yn, hin, hout, c0tok):
    P = cx.P
    with P.scope():
        epi, finish = make_rmw_epi(cx, hin, hout, c0tok, "CW")
        gemm_R(cx, wv_of(w_out, 0, KB), D, lambda kb, c0, cs: yn[:, kb, c0 : c0 + cs], KB, epi, "CW_g")
        finish()


def ffn_dense(cx, w1, w3, w2, xn, hbuf, c0tok):
    P = cx.P
    quarters = [(0, 22), (22, 22), (44, 21), (65, 21)]
    with P.scope():
        mid = P.sb("F_mid", [128, 22, T], BF16)
        for q0, nq in quarters:
            with P.scope():
                wb = [P.sb(f"FU_w{i}", [128, KB, 256], BF16) for i in range(2)]

                def epi1(nb, ci, ps, c0, cs, ev_mm, js):
                    P.wait("scalar", ev_mm)
                    return P.do("scalar", lambda e: e.activation(out=mid[:js, nb, c0 : c0 + cs], in_=ps, func=AF.Silu), inc=cx.act)

                def epi3(nb, ci, ps, c0, cs, ev_mm, js):
                    P.wait("vector", ev_mm)
                    return P.do("vector", lambda e: e.tensor_tensor(
                        out=mid[:js, nb, c0 : c0 + cs], in0=mid[:js, nb, c0 : c0 + cs], in1=ps, op=ALU.mult), inc=cx.dve)

                v1 = w1.rearrange("(kb p) n -> p kb n", p=128)
                v3 = w3.rearrange("(kb p) n -> p kb n", p=128)
                c_lo = q0 * 128
                gemm_R(cx, lambda n0, ns: v1[:, :, c_lo + n0 : c_lo + n0 + ns], nq * 128,
                       lambda kb, c0, cs: xn[:, kb, c0 : c0 + cs], KB, epi1, "FU1", NW=256, wbufs=wb)
                for e in ENGS:
                    P.wait(e, cx.last)
                P.flush()
                gemm_R(cx, lambda n0, ns: v3[:, :, c_lo + n0 : c_lo + n0 + ns], nq * 128,
                       lambda kb, c0, cs: xn[:, kb, c0 : c0 + cs], KB, epi3, "FU3", NW=256, wbufs=wb)
                for e in ENGS:
                    P.wait(e, cx.last)
                P.flush()
            with P.scope():
                epi, finish = make_rmw_epi(cx, hbuf, hbuf, c0tok, "FD")
                gemm_R(cx, wv_of(w2, q0 * 128, nq), D, lambda kb, c0, cs: mid[:, kb, c0 : c0 + cs], nq, epi, "FD_g", NW=256)
                finish()


def moe_router(cx, prm, lgT, rstd, gateT):
    P = cx.P
    nb9 = [(tb * 128, min(128, T - tb * 128)) for tb in range(9)]
    with P.scope():
        rb = P.sb("R_rb", [8, 1], F32)
        lgtm = P.sb("R_lgtm", [128, 9, 8], F32)
        gtm = P.sb("R_gtm", [128, 9, 8], F32)
        m1 = P.sb("R_m1", [128, 4], F32)
        eq1 = P.sb("R_eq1", [128, 8], F32)
        eq2 = P.sb("R_eq2", [128, 8], F32)
        l2 = P.sb("R_l2", [128, 8], F32)
        load_small(cx, rb[:, :], prm["router_b"])
        cx.op("gpsimd", lambda e: e.memset(lgtm[:, :, :], 0.0))
        cx.op("vector", lambda e: e.tensor_tensor(out=lgT[:8, :], in0=lgT[:8, :], in1=rstd[:8, :], op=ALU.mult))
        cx.op("vector", lambda e: e.tensor_scalar(out=lgT[:8, :], in0=lgT[:8, :], scalar1=rb[:8, 0:1], scalar2=None, op0=ALU.add))
        ps = cx.psum[6]
        for tb, (t0, n) in enumerate(nb9):
            cx.mms([lambda e, t0=t0, n=n: e.transpose(ps[:n, 0:8], lgT[:8, t0 : t0 + n], cx.ident32[:8, :8])])
            cx.op("scalar", lambda e, tb=tb, n=n: e.copy(out=lgtm[:n, tb, :], in_=ps[:n, 0:8]))
        for tb in range(9):
            x = lgtm[:, tb, :]
            cx.op("vector", lambda e, x=x: e.tensor_reduce(out=m1[:, 0:1], in_=x, axis=AX.X, op=ALU.max))
            cx.op("vector", lambda e, x=x: e.tensor_scalar(out=eq1[:, :], in0=x, scalar1=m1[:, 0:1], scalar2=None, op0=ALU.is_equal))
            cx.op("vector", lambda e, x=x: e.scalar_tensor_tensor(out=l2[:, :], in0=eq1[:, :], scalar=-1e30, in1=x, op0=ALU.mult, op1=ALU.add))
            cx.op("vector", lambda e: e.tensor_reduce(out=m1[:, 1:2], in_=l2[:, :], axis=AX.X, op=ALU.max))
            cx.op("vector", lambda e: e.tensor_scalar(out=eq2[:, :], in0=l2[:, :], scalar1=m1[:, 1:2], scalar2=None, op0=ALU.is_equal))
            cx.op("vector", lambda e: e.tensor_tensor(out=m1[:, 2:3], in0=m1[:, 1:2], in1=m1[:, 0:1], op=ALU.subtract))
            cx.op("scalar", lambda e: e.activation(out=m1[:, 2:3], in_=m1[:, 2:3], func=AF.Exp))
            cx.op("vector", lambda e: e.tensor_scalar(out=m1[:, 3:4], in0=m1[:, 2:3], scalar1=1.0, scalar2=None, op0=ALU.add))
            cx.op("vector", lambda e: e.reciprocal(out=m1[:, 3:4], in_=m1[:, 3:4]))
            cx.op("vector", lambda e: e.tensor_tensor(out=m1[:, 2:3], in0=m1[:, 2:3], in1=m1[:, 3:4], op=ALU.mult))
            cx.op("vector", lambda e: e.tensor_scalar(out=eq1[:, :], in0=eq1[:, :], scalar1=m1[:, 3:4], scalar2=None, op0=ALU.mult))
            cx.op("vector", lambda e, tb=tb: e.scalar_tensor_tensor(
                out=gtm[:, tb, :], in0=eq2[:, :], scalar=m1[:, 2:3], in1=eq1[:, :], op0=ALU.mult, op1=ALU.add))
        for tb, (t0, n) in enumerate(nb9):
            cx.mms([lambda e, tb=tb, n=n: e.transpose(ps[:8, 0:n], gtm[:n, tb, :], cx.ident32[:n, :n])])
            cx.op("scalar", lambda e, t0=t0, n=n: e.copy(out=gateT[:8, t0 : t0 + n], in_=ps[:8, 0:n]))
        cx.sync_chain()


def ffn_moe(cx, prm, xn, lgT, rstd, hbuf, c0tok, n_exp=NE, dbg=None):
    P = cx.P
    tcs = tok_chunks()
    with P.scope():
        gateT = P.sb("M_gateT", [8, T], F32)
        selE = P.sb("M_selE", [8, 8, 128], F32)
        gb = P.sb("M_gb", [128, T], F32)
        mid = P.sb("M_mid", [128, KB, T], BF16)
        tmpg = [P.sb(f"M_tmpg{i}", [128, 512], F32) for i in range(2)]
        moe_router(cx, prm, lgT, rstd, gateT)
        if dbg is not None:
            cx.dma(lambda e: e.dma_start(out=dbg["gateT"][:, :], in_=gateT[:8, :]))
            cx.dma(lambda e: e.dma_start(out=dbg["lgT"][:, :], in_=lgT[:8, :]))
        for ex in range(NE):
            cx.op("vector", lambda e, ex=ex: e.tensor_scalar(
                out=selE[:8, ex, :], in0=cx.ones32[:8, :], scalar1=cx.ident32[:8, ex : ex + 1], scalar2=None, op0=ALU.mult))
        for ex in range(n_exp):
            for ci, (c0, cs) in enumerate(tcs):
                cx.mms([lambda e, ex=ex, c0=c0, cs=cs: e.matmul(cx.psum[6][:, :cs], selE[:8, ex, :], gateT[:8, c0 : c0 + cs], start=True, stop=True)])
                cx.op("scalar", lambda e, c0=c0, cs=cs: e.copy(out=gb[:, c0 : c0 + cs], in_=cx.psum[6][:, :cs]))
            cx.sync_chain()
            with P.scope():
                wb = [P.sb(f"MU_w{i}", [128, KB, 128], BF16) for i in range(2)]
                tg_ev = [None, None]
                cnt = [0]

                def epi1(nb, ci, ps, c0, cs, ev_mm, js):
                    P.wait("scalar", ev_mm)
                    return P.do("scalar", lambda e: e.activation(out=mid[:js, nb, c0 : c0 + cs], in_=ps, func=AF.Silu), inc=cx.act)

                def epi3(nb, ci, ps, c0, cs, ev_mm, js):
                    P.wait("vector", ev_mm)
                    tb_ = tmpg[cnt[0] % 2]
                    cnt[0] += 1
                    ev_a = P.do("vector", lambda e: e.tensor_tensor(out=tb_[:js, :cs], in0=ps, in1=gb[:js, c0 : c0 + cs], op=ALU.mult), inc=cx.dve)
                    P.wait("vector", ev_a)
                    return P.do("vector", lambda e: e.tensor_tensor(
                        out=mid[:js, nb, c0 : c0 + cs], in0=mid[:js, nb, c0 : c0 + cs], in1=tb_[:js, :cs], op=ALU.mult), inc=cx.dve)

                v1 = prm["moe_w1"][ex].rearrange("(kb p) n -> p kb n", p=128)
                v3 = prm["moe_w3"][ex].rearrange("(kb p) n -> p kb n", p=128)
                gemm_R(cx, lambda n0, ns: v1[:, :, n0 : n0 + ns], D, lambda kb, c0, cs: xn[:, kb, c0 : c0 + cs], KB, epi1, "MU1", NW=128, wbufs=wb)
                for e in ENGS:
                    P.wait(e, cx.last)
                P.flush()
                gemm_R(cx, lambda n0, ns: v3[:, :, n0 : n0 + ns], D, lambda kb, c0, cs: xn[:, kb, c0 : c0 + cs], KB, epi3, "MU3", NW=128, wbufs=wb)
                for e in ENGS:
                    P.wait(e, cx.last)
                P.flush()
                if dbg is not None and ex == 0:
                    midf = P.sb("M_midf", [128, T], F32)
                    cx.dma(lambda e: e.dma_start(out=dbg["gb"][:, :], in_=gb[:, :]))
                    for kb in range(KB):
                        cx.op("vector", lambda e, kb=kb: e.tensor_copy(out=midf[:, :], in_=mid[:, kb, :]))
                        cx.dma(lambda e, kb=kb: e.dma_start(out=dbg["mid"][kb * 128 : (kb + 1) * 128, :], in_=midf[:, :]))
                    cx.sync_chain()
                epi, finish = make_rmw_epi(cx, hbuf, hbuf, c0tok, "MD")
                gemm_R(cx, wv_of(prm["moe_w2"][ex], 0, KB), D, lambda kb, c0, cs: mid[:, kb, c0 : c0 + cs], KB, epi, "MD_g", NW=128, wbufs=wb)
                finish()


def final_norm(cx, hbuf, parC, outT, c0tok):
    P = cx.P
    tcs = tok_chunks()
    with P.scope():
        hb = P.sb("Z_hb", [128, T], F32)
        sqt = P.sb("Z_sq", [128, T], F32)
        rstd = P.sb("Z_rstd", [128, T], F32)
        for kb in range(KB):
            cx.dma(lambda e, kb=kb: e.dma_start(out=hb[:, :], in_=hbuf[kb * 128 : (kb + 1) * 128, c0tok : c0tok + T]))
            cx.op("scalar", lambda e: e.activation(out=sqt[:, :], in_=hb[:, :], func=AF.Square))
            cx.mms([lambda e, kb=kb, ci=ci, c0=c0, cs=cs: e.matmul(
                cx.psum[ci][:, :cs], cx.ones32[:, :], sqt[:, c0 : c0 + cs], start=(kb == 0), stop=(kb == KB - 1))
                for ci, (c0, cs) in enumerate(tcs)])
        for ci, (c0, cs) in enumerate(tcs):
            cx.op("scalar", lambda e, ci=ci, c0=c0, cs=cs: e.activation(
                out=rstd[:, c0 : c0 + cs], in_=cx.psum[ci][:, :cs], func=AF.Sqrt, scale=1.0 / D, bias=EPS))
        cx.op("vector", lambda e: e.reciprocal(out=rstd[:, :], in_=rstd[:, :]))
        for kb in range(KB):
            cx.dma(lambda e, kb=kb: e.dma_start(out=hb[:, :], in_=hbuf[kb * 128 : (kb + 1) * 128, c0tok : c0tok + T]))
            cx.op("vector", lambda e, kb=kb: e.scalar_tensor_tensor(
                out=sqt[:, :], in0=hb[:, :], scalar=parC[:, 112 + kb : 113 + kb], in1=rstd[:, :], op0=ALU.mult, op1=ALU.mult))
            cx.dma(lambda e, kb=kb: e.dma_start(out=outT[kb * 128 : (kb + 1) * 128, c0tok : c0tok + T], in_=sqt[:, :]))
        cx.sync_chain()


def phaseC(cx, prm, l, yT, hin, hbuf, parC, c0tok, outT=None, do_ffn=True, do_mix=True, n_exp=NE, dbg=None):
    P = cx.P
    with P.scope():
        cx.renew()
        with P.scope() if do_mix else ExitStack():
          if do_mix:
            yn = P.sb("C_yn", [128, KB, T], BF16)
            phaseC_norms(cx, yT, parC, yn, c0tok)
            phaseC_wout(cx, prm["w_out"], yn, hin, hbuf, c0tok)
        if not do_ffn:
            cx.end_phase()
        with P.scope() if do_ffn else ExitStack():
          if do_ffn:
            xn = P.sb("C_xn", [128, KB, T], BF16)
            lgT = P.sb("C_lgT", [8, T], F32)
            rstd_keep = P.sb("C_rstdk", [128, T], F32)
            hv = hbuf[:, c0tok : c0tok + T].rearrange("(kb p) t -> p kb t", p=128)
            if l % 2 == 0:
                with P.scope():
                    rmsnorm_T(cx, hv, parC[:, 64:96], xn, "C_rn")
                ffn_dense(cx, prm["ffn_w1"], prm["ffn_w3"], prm["ffn_w2"], xn, hbuf, c0tok)
            else:
                with P.scope():
                    rw = P.sb("C_rw", [128, KB, 8], F32)
                    xf = [P.sb(f"C_xf{i}", [128, T], F32) for i in range(2)]
                    load_small(cx, rw[:, :, :], prm["router_w"].rearrange("(kb p) e -> p kb e", p=128))
                    for kb in range(KB):
                        cx.op("vector", lambda e, kb=kb: e.tensor_scalar(
                            out=rw[:, kb, :], in0=rw[:, kb, :], scalar1=parC[:, 64 + kb : 65 + kb], scalar2=None, op0=ALU.mult))
                    cx.sync_chain()
                    tcs = tok_chunks()

                    def extra(kb, hb, ev_ld, evs):
                        P.wait("tensor", ev_ld)
                        for ci, (c0, cs) in enumerate(tcs):
                            ev = P.do("tensor", lambda e, ci=ci, c0=c0, cs=cs: e.matmul(
                                cx.psum[3 + ci][:8, :cs], rw[:, kb, :], hb[:, c0 : c0 + cs], start=(kb == 0), stop=(kb == KB - 1)),
                                inc=cx.pe if ci == len(tcs) - 1 else None)
                        P.wait("sync", evs)
                        return ev

                    rstd = rmsnorm_T(cx, hv, parC[:, 64:96], xn, "C_rn", extra=extra)
                    for e in ENGS:
                        P.wait(e, cx.last)
                    cx.op("vector", lambda e: e.tensor_copy(out=rstd_keep[:, :], in_=rstd[:, :]))
                    for ci, (c0, cs) in enumerate(tcs):
                        cx.op("scalar", lambda e, ci=ci, c0=c0, cs=cs: e.copy(out=lgT[:8, c0 : c0 + cs], in_=cx.psum[3 + ci][:8, :cs]))
                    cx.sync_chain()
                ffn_moe(cx, prm, xn, lgT, rstd_keep, hbuf, c0tok, n_exp=n_exp, dbg=dbg)
        if outT is not None and do_ffn:
            final_norm(cx, hbuf, parC, outT, c0tok)
        cx.end_phase()


def _dt_of(a):
    return BF16 if a.dtype == ml_dtypes.bfloat16 else F32


def build_program(im):
    nc = bass.Bass("TRN2", target_bir_lowering=False)
    prm = {k: nc.dram_tensor(k, list(v.shape), _dt_of(v), kind="ExternalInput").ap() for k, v in im.items()}
    outT = nc.dram_tensor("outT", [D, L], F32, kind="ExternalOutput").ap()
    hbuf = nc.dram_tensor("hbuf", [D, L], F32, kind="Internal").ap()
    projT = nc.dram_tensor("projT", [PINP, L], F32, kind="Internal").ap()
    yT = nc.dram_tensor("yT", [D, L], F32, kind="Internal").ap()
    prm["hyscr"] = nc.dram_tensor("hyscr", [512, L], F32, kind="Internal").ap()
    with ExitStack() as st:
        P = Prog(nc, st)
        cx = Ctx(P)
        cx.load_ident(prm["ident"])
        for l in range(2):
            pl = dict(prm)
            for k in ["parB", "hy_w1", "hy_w2", "hy_w3", "hy_wo", "hy_fpar", "hy_decay", "w_out", "w_in"]:
                pl[k] = prm[f"{k}{l}"]
            with P.scope():
                parC = P.sb("parC", [128, 144], F32)
                load_small(cx, parC[:, :], prm[f"parC{l}"])
                cx.sync_chain()
                hsrc = prm["h0T"] if l == 0 else hbuf
                for tc in range(NTC):
                    phaseA(cx, hsrc, parC[:, 0:32], pl["w_in"], projT, tc * T)
                phaseB(cx, pl, projT, yT)
                for tc in range(NTC):
                    phaseC(cx, pl, l, yT, hsrc, hbuf, parC, tc * T, outT=outT if l == 1 else None)
        cx.sync_chain()
        P.flush()
    return nc


_PROG = {}


def kernel(**inputs):
    inp = {k: np.asarray(v) for k, v in inputs.items()}
    C = constants()
    B = inp["x"].shape[0]
    shared = {}
    for k in ["C8", "S8", "C4", "S4", "CC", "SC", "zT", "negt", "negpi", "ident", "attn_bias", "attn_biasm"]:
        shared[k] = C[k]
    for l in range(2):
        lp = layer_params(inp, l)
        for k in ["parB", "hy_w1", "hy_w2", "hy_w3", "hy_wo", "hy_fpar", "hy_decay", "parC"]:
            shared[f"{k}{l}"] = lp[k]
        shared[f"w_in{l}"] = np.ascontiguousarray(inp["w_in"][l][:, C["perm"]])
        shared[f"w_out{l}"] = np.ascontiguousarray(inp["w_out"][l])
    shared["ffn_w1"] = np.ascontiguousarray(inp["ffn_w1"][0])
    shared["ffn_w3"] = np.ascontiguousarray(inp["ffn_w3"][0])
    shared["ffn_w2"] = np.ascontiguousarray(inp["ffn_w2"][0])
    shared["router_w"] = np.ascontiguousarray(inp["router_w"][0])
    shared["router_b"] = np.ascontiguousarray(inp["router_b"][0].reshape(NE, 1))
    shared["moe_w1"] = np.ascontiguousarray(inp["moe_w1"][0])
    shared["moe_w3"] = np.ascontiguousarray(inp["moe_w3"][0])
    shared["moe_w2"] = np.ascontiguousarray(inp["moe_w2"][0])
    in_maps = []
    for b in range(B):
        m = dict(shared)
        h0 = np.concatenate([inp["meta_tokens"], inp["x"][b]], axis=0)
        m["h0T"] = np.ascontiguousarray(h0.T)
        in_maps.append(m)
    if "nc" not in _PROG:
        _PROG["nc"] = build_program(in_maps[0])
    res = run_bass_kernel_spmd(_PROG["nc"], in_maps, core_ids=list(range(B)))
    out = np.empty((B, L - NM, D), np.float32)
    for b in range(B):
        out[b] = res.results[b]["outT"][:, NM:].T
    return out
```
